# Optimizing a Trainium2 kernel written in Bass

```python
import math
import jax, jax.numpy as jnp
from jax import lax
import numpy as np

D_MODEL = 1024
BATCH = 2
SEQ = 16384
DEPTH = 4

HEAD_DIM = 128
RET_HEADS = D_MODEL // (2 * HEAD_DIM)
RET_DK = HEAD_DIM
RET_DV = HEAD_DIM
HGRN_HEADS = D_MODEL // (2 * HEAD_DIM)
HGRN_DK = HEAD_DIM
HGRN_DV = HEAD_DIM
RET_WIDTH = RET_HEADS * RET_DV
HGRN_WIDTH = HGRN_HEADS * HGRN_DV
MIX_WIDTH = RET_WIDTH + HGRN_WIDTH
RET_QK_W = RET_HEADS * RET_DK
HGRN_QK_W = HGRN_HEADS * HGRN_DK
SPLIT_SIZES = (RET_QK_W, RET_QK_W, RET_WIDTH, RET_WIDTH,
               HGRN_QK_W, HGRN_QK_W, HGRN_WIDTH, HGRN_WIDTH)
IN_COLS = sum(SPLIT_SIZES)
D_FF = 2816
CONV_WIDTH = 3
CHUNK = 64
ROPE_BASE = 10000.0
EPS = 1e-6
N_MOD = 6

kernel_name = 'hymba_style_retnet_hgrn2_convffn_adaln'


def rms_norm(x, w):
    xf = x.astype(jnp.float32)
    y = xf * lax.rsqrt(jnp.mean(xf * xf, axis=-1, keepdims=True) + EPS)
    return (y * w.astype(jnp.float32)).astype(x.dtype)


def head_norm(o, w):
    H, D = o.shape[-2], o.shape[-1]
    y = o * lax.rsqrt(jnp.mean(o * o, axis=-1, keepdims=True) + EPS)
    return y * w.astype(jnp.float32).reshape(H, D)


def rotary(x, cos, sin):
    x1, x2 = jnp.split(x, 2, axis=-1)
    c = cos[None, :, None, :]
    s = sin[None, :, None, :]
    return jnp.concatenate([x1 * c - x2 * s, x1 * s + x2 * c], axis=-1)


def to_chunks(t):
    B, S, H, D = t.shape
    n = S // CHUNK
    return t.astype(jnp.float32).reshape(B, n, CHUNK, H, D).transpose(1, 0, 3, 2, 4)


def from_chunks(t):
    n, B, H, C, D = t.shape
    return t.transpose(1, 0, 3, 2, 4).reshape(B, n * C, H, D)


def retention_chunkwise(q, k, v, log_gamma):
    qc, kc, vc = to_chunks(q), to_chunks(k), to_chunks(v)
    B, H, DK, DV = qc.shape[1], qc.shape[2], qc.shape[4], vc.shape[4]
    pos = jnp.arange(CHUNK, dtype=jnp.float32)
    lg = log_gamma.astype(jnp.float32)[:, None]
    rel = pos[:, None] - pos[None, :]
    decay_mask = jnp.where(rel[None] >= 0, jnp.exp(lg[:, :, None] * jnp.maximum(rel, 0.0)[None]), 0.0)
    q_decay = jnp.exp(lg * (pos + 1.0))[None, :, :, None]
    k_decay = jnp.exp(lg * (CHUNK - 1.0 - pos))[None, :, :, None]
    chunk_decay = jnp.exp(lg[:, 0] * CHUNK)[None, :, None, None]

    def step(state, inp):
        qi, ki, vi = inp
        scores = jnp.einsum('bhid,bhjd->bhij', qi, ki) * decay_mask[None]
        intra = jnp.einsum('bhij,bhjv->bhiv', scores, vi)
        inter = jnp.einsum('bhid,bhdv->bhiv', qi, state) * q_decay
        new_state = state * chunk_decay + jnp.einsum('bhjd,bhjv->bhdv', ki * k_decay, vi)
        return new_state, intra + inter

    init = jnp.zeros((B, H, DK, DV), jnp.float32)
    _, out = lax.scan(step, init, (qc, kc, vc))
    return from_chunks(out)


def hgrn2_chunkwise(q, log_f, k, v):
    qc, lfc, kc, vc = to_chunks(q), to_chunks(log_f), to_chunks(k), to_chunks(v)
    B, H, DK, DV = qc.shape[1], qc.shape[2], qc.shape[4], vc.shape[4]
    causal = jnp.tril(jnp.ones((CHUNK, CHUNK), dtype=bool))[:, :, None]

    def step(state, inp):
        qi, lfi, ki, vi = inp
        b = jnp.cumsum(lfi, axis=-2)
        diff = b[:, :, :, None, :] - b[:, :, None, :, :]
        decay = jnp.where(causal, jnp.exp(jnp.where(causal, diff, 0.0)), 0.0)
        scores = jnp.sum(qi[:, :, :, None, :] * ki[:, :, None, :, :] * decay, axis=-1)
        intra = jnp.einsum('bhij,bhjv->bhiv', scores, vi)
        inter = jnp.einsum('bhid,bhdv->bhiv', qi * jnp.exp(b), state)
        b_last = b[:, :, -1:, :]
        new_state = state * jnp.exp(b_last)[:, :, 0, :, None] + jnp.einsum('bhjd,bhjv->bhdv', ki * jnp.exp(b_last - b), vi)
        return new_state, intra + inter

    init = jnp.zeros((B, H, DK, DV), jnp.float32)
    _, out = lax.scan(step, init, (qc, lfc, kc, vc))
    return from_chunks(out)


def hybrid_mixer(h, w_in, w_out, ret_norm_w, hgrn_norm_w, lb, cos, sin, log_gamma):
    B, S, _ = h.shape
    proj = h @ w_in
    idx = np.cumsum(SPLIT_SIZES)[:-1].tolist()
    rq, rk, rv, rg, hq, hf, hi, hg = jnp.split(proj, idx, axis=-1)

    rq = rotary(rq.reshape(B, S, RET_HEADS, RET_DK).astype(jnp.float32), cos, sin)
    rk = rotary(rk.reshape(B, S, RET_HEADS, RET_DK).astype(jnp.float32), cos, sin) * (RET_DK ** -0.5)
    rv = rv.reshape(B, S, RET_HEADS, RET_DV)
    r_out = retention_chunkwise(rq, rk, rv, log_gamma)
    r_out = head_norm(r_out, ret_norm_w) * jax.nn.silu(rg.astype(jnp.float32).reshape(B, S, RET_HEADS, RET_DV))

    z = hf.astype(jnp.float32).reshape(B, S, HGRN_HEADS, HGRN_DK)
    lbh = lb.astype(jnp.float32).reshape(HGRN_HEADS, HGRN_DK)
    log_f = jax.nn.log_sigmoid(z) + jnp.log1p(lbh * jnp.exp(-z))
    k_in = (1.0 - lbh) * jax.nn.sigmoid(-z)
    q_h = jax.nn.silu(hq.astype(jnp.float32).reshape(B, S, HGRN_HEADS, HGRN_DK))
    v_h = hi.reshape(B, S, HGRN_HEADS, HGRN_DV)
    g_out = hgrn2_chunkwise(q_h, log_f, k_in, v_h)
    g_out = head_norm(g_out, hgrn_norm_w) * jax.nn.silu(hg.astype(jnp.float32).reshape(B, S, HGRN_HEADS, HGRN_DV))

    merged = jnp.concatenate([r_out.reshape(B, S, RET_WIDTH), g_out.reshape(B, S, HGRN_WIDTH)], axis=-1)
    return merged.astype(h.dtype) @ w_out


def conv_ffn(h, w_gate, w_val, conv_w, conv_b, w_down):
    a = h @ w_gate
    S = a.shape[1]
    ap = jnp.pad(a, ((0, 0), (CONV_WIDTH - 1, 0), (0, 0)))
    acc = conv_b[None, None, :] + ap[:, 0:S] * conv_w[0]
    for j in range(1, CONV_WIDTH):
        acc = acc + ap[:, j:j + S] * conv_w[j]
    return (jax.nn.silu(acc) * (h @ w_val)) @ w_down


def setup_inputs(seed: int = 0) -> dict:
    key = jax.random.key(seed)
    ks = jax.random.split(key, 20)
    f32 = jnp.float32
    nrm = lambda k, shape, s: (jax.random.normal(k, shape, f32) * s).astype(f32)
    return {
        'x': nrm(ks[0], (BATCH, SEQ, D_MODEL), 1.0),
        'c': nrm(ks[1], (BATCH, D_MODEL), 1.0),
        'w_in': nrm(ks[2], (DEPTH, D_MODEL, IN_COLS), D_MODEL ** -0.5),
        'w_out': nrm(ks[3], (DEPTH, MIX_WIDTH, D_MODEL), MIX_WIDTH ** -0.5),
        'ret_norm_w': 1.0 + nrm(ks[4], (DEPTH, RET_WIDTH), 0.02),
        'hgrn_norm_w': 1.0 + nrm(ks[5], (DEPTH, HGRN_WIDTH), 0.02),
        'hgrn_lb_logits': nrm(ks[6], (DEPTH, HGRN_HEADS * HGRN_DK), 0.1),
        'norm1_w': 1.0 + nrm(ks[7], (DEPTH, D_MODEL), 0.02),
        'norm2_w': 1.0 + nrm(ks[8], (DEPTH, D_MODEL), 0.02),
        'ada_w': nrm(ks[9], (DEPTH, D_MODEL, N_MOD * D_MODEL), 0.5 * D_MODEL ** -0.5),
        'ada_b': nrm(ks[10], (DEPTH, N_MOD * D_MODEL), 0.01),
        'w_gate': nrm(ks[11], (DEPTH, D_MODEL, D_FF), D_MODEL ** -0.5),
        'w_val': nrm(ks[12], (DEPTH, D_MODEL, D_FF), D_MODEL ** -0.5),
        'conv_w': nrm(ks[13], (DEPTH, CONV_WIDTH, D_FF), CONV_WIDTH ** -0.5),
        'conv_b': nrm(ks[14], (DEPTH, D_FF), 0.01),
        'w_down': nrm(ks[15], (DEPTH, D_FF, D_MODEL), D_FF ** -0.5),
        'final_norm_w': 1.0 + nrm(ks[16], (D_MODEL,), 0.02),
    }


def reference(x, c, w_in, w_out, ret_norm_w, hgrn_norm_w, hgrn_lb_logits, norm1_w, norm2_w,
              ada_w, ada_b, w_gate, w_val, conv_w, conv_b, w_down, final_norm_w):
    S = x.shape[1]
    pos = jnp.arange(S, dtype=jnp.float32)
    inv_freq = ROPE_BASE ** (-jnp.arange(0, RET_DK, 2, dtype=jnp.float32) / RET_DK)
    ang = pos[:, None] * inv_freq[None, :]
    cos, sin = jnp.cos(ang), jnp.sin(ang)
    log_gamma = jnp.log(1.0 - jnp.exp2(-5.0 - jnp.arange(RET_HEADS, dtype=jnp.float32)))
    p = jax.nn.softmax(hgrn_lb_logits.astype(jnp.float32), axis=0)
    lower_bounds = jnp.cumsum(p, axis=0) - p[0:1]
    c_act = jax.nn.silu(c)

    for l in range(DEPTH):
        mod = (c_act @ ada_w[l] + ada_b[l])[:, None, :]
        sh1, sc1, g1, sh2, sc2, g2 = jnp.split(mod, N_MOD, axis=-1)
        h = rms_norm(x, norm1_w[l]) * (1.0 + sc1) + sh1
        x = x + g1 * hybrid_mixer(h, w_in[l], w_out[l], ret_norm_w[l], hgrn_norm_w[l],
                                  lower_bounds[l], cos, sin, log_gamma)
        h = rms_norm(x, norm2_w[l]) * (1.0 + sc2) + sh2
        x = x + g2 * conv_ffn(h, w_gate[l], w_val[l], conv_w[l], conv_b[l], w_down[l])

    return rms_norm(x, final_norm_w)
```

```python
import contextlib
import numpy as np
import concourse.bass as bass
import concourse.mybir as mybir
from concourse.bass_utils import run_bass_kernel_spmd

F32 = mybir.dt.float32
BF16 = mybir.dt.bfloat16
ALU = mybir.AluOpType
AF = mybir.ActivationFunctionType
AX = mybir.AxisListType

D = 1024
DFF = 2816
NFC = DFF // 128
SEQ = 16384
NSEG = 4
LSEG = SEQ // NSEG
DEPTH = 4
EPS = 1e-6
SAME_ENG_SYNC = True


class Buf:
    __slots__ = ("name", "lw", "rd", "excl")

    def __init__(self, name, excl=False):
        self.name = name
        self.excl = excl
        self.lw = None
        self.rd = []


class Op:
    __slots__ = ("eng", "fn", "deps", "inc", "dma", "dsem", "val", "idx")


class Prog:
    ENGS = ("pe", "act", "dve", "pool", "sp")
    NDSEM = 8

    def __init__(self, nc):
        self.nc = nc
        self.ops = []
        self.dma_cnt = {"sp": 0, "pool": 0, "act": 0}
        self.dma_last = {}
        self.last_op = {}
        self.pending_bar = {}
        self.marks = {}

    def op(self, eng, fn, rd=(), wr=(), dma=False):
        o = Op()
        o.eng = eng
        o.fn = fn
        o.dma = dma
        o.inc = False
        o.idx = len(self.ops)
        deps = set()
        ex = [b for b in rd if b.excl]
        if ex:
            rd = [b for b in rd if not b.excl]
            wr = list(wr) + ex
        for b in rd:
            if b.lw is not None:
                deps.add(b.lw)
        for b in wr:
            if b.lw is not None:
                deps.add(b.lw)
            deps.update(b.rd)
        if eng in self.pending_bar:
            deps.update(self.pending_bar.pop(eng))
        if dma:
            c = self.dma_cnt[eng]
            self.dma_cnt[eng] = c + 1
            o.dsem = (eng, c % self.NDSEM)
            o.val = 16 * (c // self.NDSEM + 1)
            prev = self.dma_last.get(o.dsem)
            if prev is not None:
                deps.add(prev)
            self.dma_last[o.dsem] = o.idx
        fd = set()
        for d in deps:
            od = self.ops[d]
            if not od.dma and od.eng == eng:
                if eng == "pe" or not SAME_ENG_SYNC:
                    continue
            fd.add(d)
            od.inc = True
        o.deps = fd
        for b in rd:
            b.rd.append(o.idx)
        for b in wr:
            b.lw = o.idx
            b.rd = []
        self.ops.append(o)
        if not dma:
            self.last_op[eng] = o.idx
        return o

    def barrier(self):
        pts = set(self.last_op.values()) | set(self.dma_last.values())
        for p in pts:
            self.ops[p].inc = True
        for e in self.ENGS:
            self.pending_bar[e] = set(pts) | self.pending_bar.get(e, set())

    def emit(self, es, final_waits):
        nc = self.nc
        import os
        stop = os.environ.get("KSTOP")
        if stop and stop in self.marks:
            n = self.marks[stop]
            self.ops = self.ops[:n]
            final_waits = [d for d in final_waits if d < n]
            print("KSTOP", stop, "ops", n)
        sems = {e: es.enter_context(nc.semaphore("s_" + e)) for e in self.ENGS}
        dsems = {}
        for q in ("sp", "pool", "act"):
            if self.dma_cnt[q]:
                for i in range(self.NDSEM):
                    dsems[(q, i)] = es.enter_context(nc.semaphore("d_%s%d" % (q, i)))
        cnt = {e: 0 for e in self.ENGS}
        for o in self.ops:
            if not o.dma and o.inc:
                cnt[o.eng] += 1
                o.val = cnt[o.eng]
        blk = es.enter_context(nc.Block())
        ops = self.ops

        def run(eng_name, e):
            wm = {}
            for o in ops:
                if o.eng != eng_name:
                    continue
                for d in sorted(o.deps):
                    od = ops[d]
                    key = od.dsem if od.dma else od.eng
                    if wm.get(key, 0) < od.val:
                        e.wait_ge(dsems[key] if od.dma else sems[key], od.val)
                        wm[key] = od.val
                ins = o.fn(e)
                if o.dma:
                    ins.then_inc(dsems[o.dsem], 16)
                elif o.inc:
                    ins.then_inc(sems[o.eng], 1)
            if eng_name == "sp":
                for d in final_waits:
                    od = ops[d]
                    key = od.dsem
                    if wm.get(key, 0) < od.val:
                        e.wait_ge(dsems[key], od.val)
                        wm[key] = od.val

        @blk.tensor
        def _(e):
            run("pe", e)

        @blk.scalar
        def _(e):
            run("act", e)

        @blk.vector
        def _(e):
            run("dve", e)

        @blk.gpsimd
        def _(e):
            run("pool", e)

        @blk.sync
        def _(e):
            run("sp", e)


def _consts():
    j = np.arange(128)
    gam = 1.0 - np.exp2(-5.0 - np.arange(4, dtype=np.float64))
    lg = np.log(gam)
    vtab = np.exp(-lg[None, :] * (j[:, None] + 1.0)) * (128.0 ** -0.5)
    gout = np.exp(lg[None, :] * (j[:, None] + 1.0))
    gC = np.broadcast_to(np.exp(lg * 128.0)[None, :], (128, 4))
    mask = (j[:, None] <= j[None, :]).astype(np.float32)
    ch = j // 64
    same = ch[:, None] == ch[None, :]
    hmask = (same & (j[:, None] <= j[None, :])).astype(np.float32)
    mid = ch * 64 + 31
    Wc = (same * ((j[:, None] <= j[None, :]).astype(np.float64) - (j[:, None] <= mid[None, :]).astype(np.float64)))
    Wcol = np.zeros((128, 4))
    Wcol[:, 0] = (j <= 31)
    Wcol[:, 1] = (j < 64)
    Wcol[:, 2] = (j >= 64) & (j <= 95)
    Wcol[:, 3] = (j >= 64)
    ident = np.eye(128)
    cst = np.concatenate([vtab, gout, gC, mask, hmask, Wc, Wcol, ident, np.ones((128, 128))], axis=1).astype(np.float32)
    return cst


C_VTAB, C_GOUT, C_GC, C_MASK, C_HMASK, C_WC, C_WCOL, C_ID, C_ONES = 0, 4, 8, 12, 140, 268, 396, 400, 528
C_TOT = 656


def _rope_tables():
    pos = np.arange(SEQ, dtype=np.float32)
    inv = (np.float32(10000.0) ** (-np.arange(0, 128, 2, dtype=np.float32) / np.float32(128))).astype(np.float32)
    ang = (pos[:, None] * inv[None, :]).astype(np.float32)
    return np.cos(ang).astype(np.float32), np.sin(ang).astype(np.float32)


def build_seg(L):
    NS = L // 128
    NB = min(NS, 16)
    NBLK = NS // NB
    TF = min(L, 1024)
    NTF = L // TF
    SF = TF // 128
    NH = TF // 512

    nc = bass.Bass("TRN2", target_bir_lowering=False)

    def din(name, shape):
        return nc.dram_tensor(name, shape, F32, kind="ExternalInput").ap()

    def dout(name, shape):
        return nc.dram_tensor(name, shape, F32, kind="ExternalOutput").ap()

    x_d = din("x", [L, D])
    ccol_d = din("ccol", [128, 8])
    w_in_d = din("w_in", [D, 4096])
    w_out_d = din("w_out", [D, D])
    w_gate_d = din("w_gate", [D, DFF])
    w_val_d = din("w_val", [D, DFF])
    w_down_d = din("w_down", [DFF, D])
    ada_w_d = din("ada_w", [D, 6 * D])
    ada_b_d = din("ada_b", [1, 6 * D])
    adabcol_d = din("adabcol", [128, 48])
    n1col_d = din("n1col", [128, 8])
    n2col_d = din("n2col", [128, 8])
    retnw_d = din("retnw", [1, 512])
    hgnw_d = din("hgnw", [1, 512])
    fnw_d = din("fnw", [1, D])
    lbl_d = din("lbl", [1, 4 * 512])
    sel_d = din("sel", [1, 4])
    convw_d = din("convw", [128, NFC * 3])
    convb_d = din("convb", [128, NFC])
    sret_d = din("sret", [128, 512])
    shg_d = din("shg", [128, 512])
    ahalo_d = din("ahalo", [128, NFC * 2])
    cos_d = din("cos", [L, 64])
    sin_d = din("sin", [L, 64])
    cst_d = din("cst", [128, C_TOT])
    xo_d = dout("xo", [L, D])
    yo_d = dout("yo", [L, D])
    sret_o = dout("sret_o", [128, 512])
    shg_o = dout("shg_o", [128, 512])
    ahalo_o = dout("ahalo_o", [128, NFC * 2])

    es = contextlib.ExitStack()
    with es:
        def sb(name, shape, dt=F32):
            return es.enter_context(nc.sbuf_tensor("sb_" + name, shape, dt))

        P = Prog(nc)
        BIGA = sb("BIGA", [128, NFC * 1024], BF16)
        BIGB = sb("BIGB", [128, 16384], BF16)
        BIGC = sb("BIGC", [128, 16384], BF16)
        BIGD = sb("BIGD", [128, 8192], BF16)
        XM = sb("XM", [128, 8 * 1024], F32)
        cst = sb("cst", [128, C_TOT])
        idb = sb("idb", [128, 128], BF16)
        g1bc = sb("g1bc", [128, D])
        g2bc = sb("g2bc", [128, D])
        MIXTAB = sb("mixtab", [128, 2048])
        retnw, hgnw, lbbc, omlbc = (MIXTAB[:, i * 512:(i + 1) * 512] for i in range(4))
        adabcol = sb("adabcol", [128, 48])
        cols = sb("cols", [128, 64])
        ccol = sb("ccolsb", [128, 8])
        cact = sb("cact", [128, 8], BF16)
        convw = sb("convw", [128, NFC * 3])
        convb = sb("convb", [128, NFC])
        ahalo = sb("ahalosb", [128, NFC * 2])
        Sret = sb("Sret", [128, 512])
        Shg = sb("Shg", [128, 512])
        Sbf = sb("Sbf", [128, 512], BF16)
        Srbf = sb("Srbf", [128, 512], BF16)
        cs = sb("cossin", [128, 2, 2, 64])
        small = sb("small", [128, 64])
        carg = sb("carg", [128, 4, 6])
        ecol = sb("ecol", [128, 4, 6])
        selbc = sb("selbc", [128, 4])
        abuf = sb("abuf", [128, 2, 516])
        bft = sb("bft", [128, 8 * 512], BF16)
        lbt = XM[:, 0:2048]
        lbm = XM[:, 2048:2560]
        cactbc = bft[:, 0:1024].rearrange("p (k c) -> p k c", k=8)
        accb = MIXTAB[:, 0:1024].rearrange("p (a b) -> p a b", a=2)
        sgb = MIXTAB[:, 1024:2048].rearrange("p (a b) -> p a b", a=2)
        fnwbc = g1bc

        PS = [es.enter_context(nc.psum_tensor("ps%d" % i, [128, 512], F32)) if i != 4 else None for i in range(8)]
        PSBF = es.enter_context(nc.psum_tensor("psbf", [128, 1024], BF16))

        bank = [Buf("bank%d" % i, excl=True) for i in range(8)]

        def B(n):
            return Buf(n)

        def xm(i):
            return XM[:, i * 1024:(i + 1) * 1024]

        def xh(i):
            return XM[:, i * 1024:i * 1024 + 512]

        def xh2(i):
            return XM[:, i * 1024 + 512:(i + 1) * 1024]

        def bt(i):
            return bft[:, i * 512:(i + 1) * 512]

        def c_(off, n):
            return cst[:, off:off + n]

        def h4(ap):
            return ap.rearrange("p (h d) -> p h d", h=4)

        def bc_last(ap4):
            return ap4.unsqueeze(2).broadcast_to([128, 4, 128])

        def bc_mid(ap128):
            return ap128.unsqueeze(1).broadcast_to([128, 4, 128])

        def dma(q, out, in_, rd=(), wr=()):
            return P.op(q, lambda e: e.dma_start(out=out, in_=in_), rd=rd, wr=wr, dma=True)

        def mm(out, lhsT, rhs, start, stop, rd, wr, tp=None):
            if tp is None:
                return P.op("pe", lambda e: e.matmul(out, lhsT=lhsT, rhs=rhs, start=start, stop=stop), rd=rd, wr=wr)
            return P.op("pe", lambda e: e.matmul(out, lhsT=lhsT, rhs=rhs, start=start, stop=stop, tile_position=tp), rd=rd, wr=wr)

        def tr(out, in_, ident, rd, wr):
            return P.op("pe", lambda e: e.transpose(out=out, in_=in_, identity=ident), rd=rd, wr=wr)

        def act(out, in_, func, rd, wr, **kw):
            return P.op("act", lambda e: e.activation(out=out, in_=in_, func=func, **kw), rd=rd, wr=wr)

        def tt(eng, out, in0, in1, op, rd, wr):
            return P.op(eng, lambda e: e.tensor_tensor(out=out, in0=in0, in1=in1, op=op), rd=rd, wr=wr)

        def ts(eng, out, in0, s1, s2, op0, op1, rd, wr):
            if s2 is None:
                return P.op(eng, lambda e: e.tensor_scalar(out=out, in0=in0, scalar1=s1, scalar2=None, op0=op0), rd=rd, wr=wr)
            return P.op(eng, lambda e: e.tensor_scalar(out=out, in0=in0, scalar1=s1, scalar2=s2, op0=op0, op1=op1), rd=rd, wr=wr)

        def stt(eng, out, in0, sc, in1, op0, op1, rd, wr, tmp=None, b_tmp=None):
            return P.op(eng, lambda e: e.scalar_tensor_tensor(out=out, in0=in0, scalar=sc, in1=in1, op0=op0, op1=op1), rd=rd, wr=wr)

        def rsq(col, rd, wr):
            act(col, col, AF.Sqrt, rd, wr)
            return P.op("dve", lambda e: e.reciprocal(out=col, in_=col), rd=wr, wr=wr)

        def cp(eng, out, in_, rd, wr):
            if eng == "act":
                return P.op("act", lambda e: e.copy(out=out, in_=in_), rd=rd, wr=wr)
            return P.op(eng, lambda e: e.tensor_copy(out=out, in_=in_), rd=rd, wr=wr)

        b_cst = B("cst")
        b_misc = B("misc")
        dma("sp", cst[:], cst_d, wr=[b_cst])
        dma("sp", ccol[:], ccol_d, wr=[b_misc])
        dma("sp", cols[:, 48:56], n1col_d, wr=[b_misc])
        dma("sp", cols[:, 56:64], n2col_d, wr=[b_misc])
        dma("sp", adabcol[:], adabcol_d, wr=[b_misc])
        dma("sp", g1bc[:], ada_b_d[0:1, 2048:3072].partition_broadcast(128), wr=[b_misc])
        dma("sp", g2bc[:], ada_b_d[0:1, 5120:6144].partition_broadcast(128), wr=[b_misc])
        dma("sp", convw[:], convw_d, wr=[b_misc])
        dma("sp", convb[:], convb_d, wr=[b_misc])
        dma("sp", ahalo[:], ahalo_d, wr=[b_misc])
        dma("sp", Sret[:], sret_d, wr=[b_misc])
        dma("sp", Shg[:], shg_d, wr=[b_misc])
        dma("sp", retnw, retnw_d.partition_broadcast(128), wr=[b_misc])
        dma("sp", hgnw, hgnw_d.partition_broadcast(128), wr=[b_misc])
        dma("sp", lbt, lbl_d.partition_broadcast(128), wr=[b_misc])
        dma("sp", selbc[:], sel_d.partition_broadcast(128), wr=[b_misc])
        P.barrier()
        cp("dve", idb[:], c_(C_ID, 128), rd=[b_cst], wr=[b_misc])
        l4 = lbt.rearrange("p (m f) -> p m f", m=4)
        tt("dve", lbm, l4[:, 0, :], l4[:, 1, :], ALU.max, [b_misc], [b_misc])
        tt("dve", lbm, lbm, l4[:, 2, :], ALU.max, [b_misc], [b_misc])
        tt("dve", lbm, lbm, l4[:, 3, :], ALU.max, [b_misc], [b_misc])
        tt("dve", l4, l4, lbm.unsqueeze(1).broadcast_to([128, 4, 512]), ALU.subtract, [b_misc], [b_misc])
        act(lbt, lbt, AF.Exp, [b_misc], [b_misc])
        tt("dve", lbm, l4[:, 0, :], l4[:, 1, :], ALU.add, [b_misc], [b_misc])
        tt("dve", lbm, lbm, l4[:, 2, :], ALU.add, [b_misc], [b_misc])
        tt("dve", lbm, lbm, l4[:, 3, :], ALU.add, [b_misc], [b_misc])
        P.op("dve", lambda e: e.reciprocal(out=lbm, in_=lbm), rd=[b_misc], wr=[b_misc])
        ts("dve", lbbc, l4[:, 0, :], selbc[:, 0:1], None, ALU.mult, None, [b_misc], [b_misc])
        for m in range(1, 4):
            stt("dve", lbbc, l4[:, m, :], selbc[:, m:m + 1], lbbc, ALU.mult, ALU.add, [b_misc], [b_misc], XM[:, 3072:3584], b_misc)
        tt("dve", lbbc, lbbc, lbm, ALU.mult, [b_misc], [b_misc])
        ts("dve", omlbc, lbbc, -1.0, 1.0, ALU.mult, ALU.add, [b_misc], [b_misc])
        act(cact[:], ccol[:], AF.Silu, [b_misc], [b_misc])
        for kc in range(8):
            cp("dve", cactbc[:, kc, :], cact[:, kc:kc + 1].broadcast_to([128, 128]), [b_misc], [b_misc])
        b_slab = [B("slab%d" % i) for i in range(4)]
        slabs = [BIGC[:, i * 4096:(i + 1) * 4096].rearrange("p (k c) -> p k c", k=8) for i in range(4)]
        vbase = {0: 0, 1: 8, 3: 16, 4: 24}
        for g in range(12):
            sl = g % 4
            dma("pool", slabs[sl], ada_w_d[:, g * 512:(g + 1) * 512].rearrange("(k p) c -> p k c", p=128), wr=[b_slab[sl]])
            v = (g * 512) // 1024
            if v in (2, 5):
                cg = g % 2
                tab = g1bc if v == 2 else g2bc
                for kc in range(8):
                    mm(PS[2 + cg][:, :], cactbc[:, kc, :], slabs[sl][:, kc, :], kc == 0, kc == 7, [b_slab[sl], b_misc], [bank[2 + cg]])
                tt("dve", tab[:, cg * 512:(cg + 1) * 512], PS[2 + cg][:, :], tab[:, cg * 512:(cg + 1) * 512], ALU.add, [bank[2 + cg], b_misc], [b_misc])
            else:
                for cc in range(4):
                    ci = vbase[v] + ((g * 512) % 1024) // 128 + cc
                    for kc in range(8):
                        mm(PS[1][:, ci:ci + 1], slabs[sl][:, kc, cc * 128:(cc + 1) * 128], cact[:, kc:kc + 1], kc == 0, kc == 7, [b_slab[sl], b_misc], [bank[1]])
        tt("dve", cols[:, 0:32], PS[1][:, 0:32], adabcol[:, 0:32], ALU.add, [bank[1], b_misc], [b_misc])
        stt("dve", cols[:, 32:40], cols[:, 8:16], 1.0, cols[:, 48:56], ALU.add, ALU.mult, [b_misc], [b_misc], small[:, 16:24], b_misc)
        stt("dve", cols[:, 40:48], cols[:, 24:32], 1.0, cols[:, 56:64], ALU.add, ALU.mult, [b_misc], [b_misc], small[:, 16:24], b_misc)
        P.barrier()

        P.marks["pro"] = len(P.ops)
        sh1c, w1c, sh2c, w2c = cols[:, 0:8], cols[:, 32:40], cols[:, 16:24], cols[:, 40:48]
        identf = c_(C_ID, 128)

        P.op("dve", lambda e: e.memset(PS[5][:, :], 0.0), wr=[bank[5]])

        def norm_transpose(xt, b_x, dstT, b_dst, tok0, shc, wc):
            junk = xm(7)
            b_j = b_tr[7]
            act(junk, xt, AF.Square, [b_x], [b_j, b_sm], accum_out=small[:, 0:1])
            ts("dve", small[:, 1:2], small[:, 0:1], 1.0 / D, EPS, ALU.mult, ALU.add, [b_sm], [b_sm])
            rsq(small[:, 1:2], [b_sm], [b_sm])
            act(junk, xt, AF.Identity, [b_x, b_sm], [b_j], scale=small[:, 1:2])
            for hb in range(2):
                for q in range(4):
                    kc = hb * 4 + q
                    tr(PS[hb][:, q * 128:(q + 1) * 128], junk[:, kc * 128:(kc + 1) * 128], identf, [b_j, b_cst], [bank[hb]])
                for q in range(4):
                    kc = hb * 4 + q
                    act(dstT[:, kc, tok0:tok0 + 128], PS[hb][:, q * 128:(q + 1) * 128], AF.Identity, [bank[hb], b_misc], [b_dst],
                        scale=wc[:, kc:kc + 1], bias=shc[:, kc:kc + 1])

        b_tr = [B("tr%d" % i) for i in range(8)]
        b_bt = [B("bt%d" % i) for i in range(8)]
        b_sm = B("small")
        b_S = B("S")
        b_Sbf = B("Sbf")
        b_SrA = b_Sbf
        b_SrB = B("SrB")
        b_cs = [B("cs0"), B("cs1")]
        b_ec = B("ecol")

        def head_norm_merge(o_sb, b_o, gate, b_g, mT, b_mT, kc0, tok0):
            sq = xh(6)
            tt("pool", sq, o_sb, o_sb, ALU.mult, [b_o], [b_tr[6]])
            P.op("dve", lambda e: e.reduce_sum(out=small[:, 8:12], in_=h4(sq), axis=AX.X), rd=[b_tr[6]], wr=[b_sm])
            ts("dve", small[:, 12:16], small[:, 8:12], 1.0 / 128, EPS, ALU.mult, ALU.add, [b_sm], [b_sm])
            rsq(small[:, 12:16], [b_sm], [b_sm])
            tt("dve", h4(sq), h4(o_sb), bc_last(small[:, 12:16]), ALU.mult, [b_o, b_sm], [b_tr[6]])
            mg = bt(7)
            tt("pool", mg, sq, gate, ALU.mult, [b_tr[6], b_g], [b_bt[7]])
            for h in range(4):
                tr(PSBF[:, h * 128:(h + 1) * 128], mg[:, h * 128:(h + 1) * 128], idb[:], [b_bt[7], b_misc], [bank[4]])
            cp("act", mT[:, kc0:kc0 + 4, tok0:tok0 + 128], PSBF[:, 0:512].rearrange("p (h d) -> p h d", h=4), [bank[4]], [b_mT])


        final_waits = []

        for blk_i in range(NBLK):
            T0 = blk_i * NB * 128
            hT = BIGA[:, 0:8 * NB * 128].rearrange("p (k t) -> p k t", k=8)
            mT = BIGB[:, 0:8 * NB * 128].rearrange("p (k t) -> p k t", k=8)
            b_hT = [B("hT%d" % s) for s in range(NB)]
            b_mT = [B("mT%d" % s) for s in range(NB)]
            b_xs = [B("xs0"), B("xs1")]
            for s in range(NB):
                xt = xm(s % 2)
                dma("sp", xt, x_d[T0 + s * 128:T0 + (s + 1) * 128, :], wr=[b_tr[s % 2]])
                norm_transpose(xt, b_tr[s % 2], hT, b_hT[s], s * 128, sh1c, w1c)
            P.marks.setdefault("pre", len(P.ops))
            for grp in range(2):
                for ti in range(4):
                    col0 = grp * 2048 + ti * 512
                    dma("pool", slabs[ti], w_in_d[:, col0:col0 + 512].rearrange("(k p) c -> p k c", p=128), wr=[b_slab[ti]])
                if grp == 0:
                    cp("act", Sbf[:], Sret[:], [b_misc, b_S], [b_Sbf])
                else:
                    P.marks.setdefault("ret", len(P.ops))
                for s in range(NB):
                    tok = slice(s * 128, (s + 1) * 128)
                    for kc in range(8):
                        for ti in range(4):
                            mm(PS[ti][:, :], hT[:, kc, tok], slabs[ti][:, kc, :], kc == 0, kc == 7, [b_hT[s], b_slab[ti]], [bank[ti]])
                    if grp == 0:
                        g0 = T0 + s * 128
                        sl = s % 2
                        dma("sp", cs[:, sl, 0, :], cos_d[g0:g0 + 128, :], wr=[b_cs[sl]])
                        dma("sp", cs[:, sl, 1, :], sin_d[g0:g0 + 128, :], wr=[b_cs[sl]])
                        cosb = cs[:, sl, 0, :].unsqueeze(1).broadcast_to([128, 4, 64])
                        sinb = cs[:, sl, 1, :].unsqueeze(1).broadcast_to([128, 4, 64])
                        qk_sb = [xh(2), xh(3)]
                        for w_, (eng, dst) in enumerate((("dve", bt(0)), ("pool", bt(1)))):
                            cp("act", qk_sb[w_], PS[w_][:, :], [bank[w_]], [b_tr[2 + w_]])
                            src = qk_sb[w_].rearrange("p (h t d) -> p h t d", h=4, t=2)
                            x1, x2 = src[:, :, 0, :], src[:, :, 1, :]
                            d4 = dst.rearrange("p (h t d) -> p h t d", h=4, t=2)
                            t1 = xh2(2 + w_)[:, 0:256].rearrange("p (h d) -> p h d", h=4)
                            t2 = xh2(2 + w_)[:, 256:512].rearrange("p (h d) -> p h d", h=4)
                            bT_ = b_tr[2 + w_]
                            rdl = [b_tr[2 + w_], b_cs[sl]]
                            tt(eng, t1, x1, cosb, ALU.mult, rdl, [bT_])
                            tt(eng, t2, x2, sinb, ALU.mult, rdl, [bT_])
                            tt(eng, d4[:, :, 0, :], t1, t2, ALU.subtract, [bT_], [b_bt[w_]])
                            tt(eng, t1, x1, sinb, ALU.mult, rdl, [bT_])
                            tt(eng, t2, x2, cosb, ALU.mult, rdl, [bT_])
                            tt(eng, d4[:, :, 1, :], t1, t2, ALU.add, [bT_], [b_bt[w_]])
                        qr, kr = bt(0), bt(1)
                        Vp = bt(2)
                        tt("dve", h4(Vp), h4(PS[2][:, :]), bc_last(c_(C_VTAB, 4)), ALU.mult, [bank[2], b_cst], [b_bt[2]])
                        gate = xh(5)
                        act(gate, PS[3][:, :], AF.Silu, [bank[3]], [b_tr[5]])
                        tt("pool", gate, gate, retnw, ALU.mult, [b_tr[5], b_misc], [b_tr[5]])
                        for h in range(4):
                            tr(PSBF[:, h * 128:(h + 1) * 128], qr[:, h * 128:(h + 1) * 128], idb[:], [b_bt[0], b_misc], [bank[4]])
                        for h in range(4):
                            tr(PSBF[:, 512 + h * 128:512 + (h + 1) * 128], kr[:, h * 128:(h + 1) * 128], idb[:], [b_bt[1], b_misc], [bank[4]])
                        qT, kT = bt(3), bt(4)
                        cp("act", qT, PSBF[:, 0:512], [bank[4]], [b_bt[3]])
                        cp("dve", kT, PSBF[:, 512:1024], [bank[4]], [b_bt[4]])
                        for h in range(4):
                            hs = slice(h * 128, (h + 1) * 128)
                            mm(PS[5][:, hs], kT[:, hs], qT[:, hs], True, True, [b_bt[3], b_bt[4]], [bank[5]])
                        sc = bt(5)
                        tt("dve", h4(sc), h4(PS[5][:, :]), bc_mid(c_(C_MASK, 128)), ALU.mult, [bank[5], b_cst], [b_bt[5]])
                        for h in range(4):
                            hs = slice(h * 128, (h + 1) * 128)
                            mm(PS[6][:, hs], sc[:, hs], Vp[:, hs], True, False, [b_bt[5], b_bt[2]], [bank[6]])
                            mm(PS[6][:, hs], qT[:, hs], Sbf[:, hs], False, True, [b_bt[3], b_Sbf], [bank[6]])
                        o_sb = xh(2)
                        tt("dve", h4(o_sb), h4(PS[6][:, :]), bc_last(c_(C_GOUT, 4)), ALU.mult, [bank[6], b_cst], [b_tr[2]])
                        for h in range(4):
                            hs = slice(h * 128, (h + 1) * 128)
                            mm(PS[7][:, hs], kr[:, hs], Vp[:, hs], True, True, [b_bt[1], b_bt[2]], [bank[7]])
                        tt("dve", Sret[:], Sret[:], PS[7][:, :], ALU.add, [bank[7], b_S], [b_S])
                        tt("pool", h4(Sret[:]), h4(Sret[:]), bc_last(c_(C_GC, 4)), ALU.mult, [b_S, b_cst], [b_S])
                        cp("act", Sbf[:], Sret[:], [b_S], [b_Sbf])
                        head_norm_merge(o_sb, b_tr[2], gate, b_tr[5], mT, b_mT[s], 0, s * 128)
                    else:
                        qs = xh(2)
                        act(qs, PS[0][:, :], AF.Silu, [bank[0]], [b_tr[2]])
                        f_ = xh(3)
                        act(f_, PS[1][:, :], AF.Sigmoid, [bank[1]], [b_tr[3]])
                        V = bt(2)
                        cp("act", V, PS[2][:, :], [bank[2]], [b_bt[2]])
                        gate = xh(5)
                        act(gate, PS[3][:, :], AF.Silu, [bank[3]], [b_tr[5]])
                        tt("pool", gate, gate, hgnw, ALU.mult, [b_tr[5], b_misc], [b_tr[5]])
                        tt("pool", f_, f_, omlbc, ALU.mult, [b_tr[3], b_misc], [b_tr[3]])
                        tt("pool", f_, f_, lbbc, ALU.add, [b_tr[3], b_misc], [b_tr[3]])
                        lf = xh(4)
                        act(lf, f_, AF.Ln, [b_tr[3]], [b_tr[4]])
                        ts("pool", f_, f_, -1.0, 1.0, ALU.mult, ALU.add, [b_tr[3]], [b_tr[3]])
                        mm(PS[1][:, :], c_(C_WC, 128), lf, True, True, [b_cst, b_tr[4]], [bank[1]])
                        for h in range(4):
                            mm(PS[5][:, h * 4:(h + 1) * 4], lf[:, h * 128:(h + 1) * 128], c_(C_WCOL, 4), True, True, [b_cst, b_tr[4]], [bank[5]])
                        E1, E2 = xh(0), xh(1)
                        act(E1, PS[1][:, :], AF.Exp, [bank[1]], [b_tr[0]])
                        act(E2, PS[1][:, :], AF.Exp, [bank[1]], [b_tr[1]], scale=-1.0)
                        Qh, Kh = bt(0), bt(1)
                        tt("dve", Qh, qs, E1, ALU.mult, [b_tr[2], b_tr[0]], [b_bt[0]])
                        tt("pool", Kh, f_, E2, ALU.mult, [b_tr[3], b_tr[1]], [b_bt[1]])
                        cp("dve", carg[:, :, 0:4], PS[5][:, 0:16].rearrange("p (h c) -> p h c", h=4), [bank[5]], [b_ec])
                        tt("dve", carg[:, :, 4:5], carg[:, :, 1:2], carg[:, :, 0:1], ALU.subtract, [b_ec], [b_ec])
                        tt("dve", carg[:, :, 5:6], carg[:, :, 3:4], carg[:, :, 2:3], ALU.subtract, [b_ec], [b_ec])
                        act(ecol[:], carg[:], AF.Exp, [b_ec], [b_ec])
                        for h in range(4):
                            tr(PSBF[:, h * 128:(h + 1) * 128], Qh[:, h * 128:(h + 1) * 128], idb[:], [b_bt[0], b_misc], [bank[4]])
                        for h in range(4):
                            tr(PSBF[:, 512 + h * 128:512 + (h + 1) * 128], Kh[:, h * 128:(h + 1) * 128], idb[:], [b_bt[1], b_misc], [bank[4]])
                        qT, kT = bt(3), bt(4)
                        cp("act", qT, PSBF[:, 0:512], [bank[4]], [b_bt[3]])
                        cp("dve", kT, PSBF[:, 512:1024], [bank[4]], [b_bt[4]])
                        for h in range(4):
                            o_ = h * 128
                            rdl = [b_bt[3], b_bt[4]]
                            mm(PS[5][0:32, o_:o_ + 32], kT[:, o_:o_ + 32], qT[:, o_:o_ + 32], True, True, rdl, [bank[5]])
                            mm(PS[5][0:64, o_ + 32:o_ + 64], kT[:, o_:o_ + 64], qT[:, o_ + 32:o_ + 64], True, True, rdl, [bank[5]])
                            mm(PS[5][64:96, o_ + 64:o_ + 96], kT[:, o_ + 64:o_ + 96], qT[:, o_ + 64:o_ + 96], True, True, rdl, [bank[5]], tp=(0, 64))
                            mm(PS[5][64:128, o_ + 96:o_ + 128], kT[:, o_ + 64:o_ + 128], qT[:, o_ + 96:o_ + 128], True, True, rdl, [bank[5]], tp=(0, 64))
                        sc = bt(5)
                        tt("dve", h4(sc), h4(PS[5][:, :]), bc_mid(c_(C_HMASK, 128)), ALU.mult, [bank[5], b_cst], [b_bt[5]])
                        srb = [Sbf, Srbf]
                        b_sr = [b_SrA, b_SrB]
                        for cch in range(2):
                            p0 = cch * 64
                            er = ecol[:, :, 2 * cch:2 * cch + 1].broadcast_to([128, 4, 128])
                            e1 = ecol[:, :, 2 * cch + 1:2 * cch + 2].broadcast_to([128, 4, 128])
                            e2 = ecol[:, :, 4 + cch:5 + cch].broadcast_to([128, 4, 128])
                            tt("dve", h4(srb[cch][:]), h4(Shg[:]), er, ALU.mult, [b_S, b_ec], [b_sr[cch]])
                            for h in range(4):
                                hs = slice(h * 128, (h + 1) * 128)
                                mm(PS[7][:, hs], Kh[p0:p0 + 64, hs], V[p0:p0 + 64, hs], True, True, [b_bt[1], b_bt[2]], [bank[7]])
                            tmp = xh(0)
                            tt("dve", h4(tmp), h4(PS[7][:, :]), e2, ALU.mult, [bank[7], b_ec], [b_tr[0]])
                            tt("pool", h4(Shg[:]), h4(Shg[:]), e1, ALU.mult, [b_S, b_ec], [b_S])
                            tt("pool", Shg[:], Shg[:], tmp, ALU.add, [b_S, b_tr[0]], [b_S])
                        for h in range(4):
                            hs = slice(h * 128, (h + 1) * 128)
                            mm(PS[6][:, hs], sc[:, hs], V[:, hs], True, False, [b_bt[5], b_bt[2]], [bank[6]])
                            mm(PS[6][0:64, hs], qT[:, h * 128:h * 128 + 64], Sbf[:, hs], False, False, [b_bt[3], b_SrA], [bank[6]])
                            mm(PS[6][64:128, hs], qT[:, h * 128 + 64:h * 128 + 128], Srbf[:, hs], False, True, [b_bt[3], b_SrB], [bank[6]], tp=(0, 64))
                        o_sb = xh(2)
                        cp("act", o_sb, PS[6][:, :], [bank[6]], [b_tr[2]])
                        head_norm_merge(o_sb, b_tr[2], gate, b_tr[5], mT, b_mT[s], 4, s * 128)
            P.marks.setdefault("hg", len(P.ops))
            wo = BIGD[:, :].rearrange("p (k c) -> p k c", k=8)
            b_wo = B("wo")
            dma("pool", wo, w_out_d.rearrange("(k p) c -> p k c", p=128), wr=[b_wo])
            for s in range(NB):
                tok = slice(s * 128, (s + 1) * 128)
                xt = xm(s % 2)
                g0 = T0 + s * 128
                dma("sp", xt, x_d[g0:g0 + 128, :], wr=[b_tr[s % 2]])
                for cg in range(2):
                    for kc in range(8):
                        mm(PS[2 + cg][:, :], mT[:, kc, tok], wo[:, kc, cg * 512:(cg + 1) * 512], kc == 0, kc == 7, [b_mT[s], b_wo], [bank[2 + cg]])
                    tmp = xh(2 + cg)
                    tt("dve", tmp, PS[2 + cg][:, :], g1bc[:, cg * 512:(cg + 1) * 512], ALU.mult, [bank[2 + cg], b_misc], [b_tr[2 + cg]])
                    tt("pool", xt[:, cg * 512:(cg + 1) * 512], xt[:, cg * 512:(cg + 1) * 512], tmp, ALU.add, [b_tr[2 + cg], b_tr[s % 2]], [b_tr[s % 2]])
                dma("sp", xo_d[g0:g0 + 128, :], xt, rd=[b_tr[s % 2]])
            P.barrier()
        P.marks["wout"] = len(P.ops)
        final_waits.append(dma("sp", sret_o, Sret[:], rd=[b_S]).idx)
        final_waits.append(dma("sp", shg_o, Shg[:], rd=[b_S]).idx)
        P.barrier()

        uT = BIGA[:, 0:NFC * TF].rearrange("p (f t) -> p f t", f=NFC)
        h2T = BIGD[:, 0:8 * TF].rearrange("p (k t) -> p k t", k=8)
        wd = [BIGB[:, 0:11264].rearrange("p (f c) -> p f c", f=NFC), BIGC[:, 0:11264].rearrange("p (f c) -> p f c", f=NFC)]
        gvb = [BIGB[:, 11264:11264 + 4096].rearrange("p (w k c) -> p w k c", w=2, k=8),
               BIGC[:, 11264:11264 + 4096].rearrange("p (w k c) -> p w k c", w=2, k=8)]
        dma("sp", fnwbc[:], fnw_d.partition_broadcast(128), wr=[b_misc])
        cw3 = convw[:, :].rearrange("p (f j) -> p f j", j=3)
        ah3 = ahalo[:, :].rearrange("p (f j) -> p f j", j=2)
        b_wd = [B("wd0"), B("wd1")]
        b_gv = [B("gv0"), B("gv1")]
        b_ah = B("ahalo")
        b_ab = [B("ab0"), B("ab1")]
        b_acc = [B("acc0"), B("acc1")]
        b_sg = [B("sg0"), B("sg1")]
        it = 0
        for ft in range(NTF):
            F0 = ft * TF
            b_h2 = B("h2T")
            b_uT = [B("uT%d" % i) for i in range(NH)]
            b_xm = [B("xm%d" % i) for i in range(SF)]
            for s in range(SF):
                g0 = F0 + s * 128
                xt = xm(s)
                if s == 7:
                    pass
                dma("sp", xt, xo_d[g0:g0 + 128, :], wr=[b_tr[s]])
            for s in range(SF):
                xt = xm(s)
                junk = abuf[:, :, 0:512].rearrange("p a b -> p (a b)") if False else None
                act(MIXTAB[:, 0:1024], xt, AF.Square, [b_tr[s]], [b_acc[0], b_acc[1], b_sm], accum_out=small[:, 0:1])
                ts("dve", small[:, 1:2], small[:, 0:1], 1.0 / D, EPS, ALU.mult, ALU.add, [b_sm], [b_sm])
                rsq(small[:, 1:2], [b_sm], [b_sm])
                xn = MIXTAB[:, 0:1024]
                act(xn, xt, AF.Identity, [b_tr[s], b_sm], [b_acc[0], b_acc[1]], scale=small[:, 1:2])
                for hb in range(2):
                    for q in range(4):
                        kc = hb * 4 + q
                        tr(PS[hb][:, q * 128:(q + 1) * 128], xn[:, kc * 128:(kc + 1) * 128], identf, [b_acc[0], b_acc[1], b_cst], [bank[hb]])
                    for q in range(4):
                        kc = hb * 4 + q
                        act(h2T[:, kc, s * 128:(s + 1) * 128], PS[hb][:, q * 128:(q + 1) * 128], AF.Identity, [bank[hb], b_misc], [b_h2],
                            scale=w2c[:, kc:kc + 1], bias=sh2c[:, kc:kc + 1])
            for cg in range(2):
                dma("pool", wd[cg], w_down_d[:, cg * 512:(cg + 1) * 512].rearrange("(f p) c -> p f c", p=128), wr=[b_wd[cg]])
            for j in range(NFC // 2):
                bsl = j % 2
                dma("pool", gvb[bsl][:, 0], w_gate_d[:, j * 256:(j + 1) * 256].rearrange("(k p) c -> p k c", p=128), wr=[b_gv[bsl]])
                dma("pool", gvb[bsl][:, 1], w_val_d[:, j * 256:(j + 1) * 256].rearrange("(k p) c -> p k c", p=128), wr=[b_gv[bsl]])
                for fcl in range(2):
                    fc = 2 * j + fcl
                    for hf in range(NH):
                        pg = it % 2
                        it += 1
                        tk = slice(hf * 512, (hf + 1) * 512)
                        for kc in range(8):
                            mm(PS[pg][:, :], gvb[bsl][:, 0, kc, fcl * 128:(fcl + 1) * 128], h2T[:, kc, tk], kc == 0, kc == 7, [b_gv[bsl], b_h2], [bank[pg]])
                        for kc in range(8):
                            mm(PS[2 + pg][:, :], gvb[bsl][:, 1, kc, fcl * 128:(fcl + 1) * 128], h2T[:, kc, tk], kc == 0, kc == 7, [b_gv[bsl], b_h2], [bank[2 + pg]])
                        ab = abuf[:, pg, :]
                        cp("act", ab[:, 2:514], PS[pg][:, :], [bank[pg]], [b_ab[pg]])
                        cp("pool", ab[:, 0:2], ah3[:, fc, :], [b_ah], [b_ab[pg]])
                        acc = accb[:, pg, :]
                        act(acc, PS[pg][:, :], AF.Identity, [bank[pg], b_misc], [b_acc[pg]], scale=cw3[:, fc, 2:3], bias=convb[:, fc:fc + 1])
                        stt("dve", acc, ab[:, 1:513], cw3[:, fc, 1:2], acc, ALU.mult, ALU.add, [b_ab[pg], b_misc, b_acc[pg]], [b_acc[pg]], sgb[:, pg, :], b_sg[pg])
                        stt("dve", acc, ab[:, 0:512], cw3[:, fc, 0:1], acc, ALU.mult, ALU.add, [b_ab[pg], b_misc, b_acc[pg]], [b_acc[pg]], sgb[:, pg, :], b_sg[pg])
                        cp("pool", ah3[:, fc, :], ab[:, 512:514], [b_ab[pg]], [b_ah])
                        sg = sgb[:, pg, :]
                        act(sg, acc, AF.Silu, [b_acc[pg]], [b_sg[pg]])
                        tt("dve", uT[:, fc, tk], sg, PS[2 + pg][:, :], ALU.mult, [b_sg[pg], bank[2 + pg]], [b_uT[hf]])
            for s in range(SF):
                g0 = F0 + s * 128
                xt = xm(s)
                for cg in range(2):
                    for fc in range(NFC):
                        mm(PS[5 + cg][:, :], uT[:, fc, s * 128:(s + 1) * 128], wd[cg][:, fc, :], fc == 0, fc == NFC - 1,
                           [b_uT[(s * 128) // 512], b_wd[cg]], [bank[5 + cg]])
                    tmp = sgb[:, cg, :]
                    tt("dve", tmp, PS[5 + cg][:, :], g2bc[:, cg * 512:(cg + 1) * 512], ALU.mult, [bank[5 + cg], b_misc], [b_sg[cg]])
                    tt("pool", xt[:, cg * 512:(cg + 1) * 512], xt[:, cg * 512:(cg + 1) * 512], tmp, ALU.add, [b_sg[cg], b_tr[s]], [b_tr[s]])
                final_waits.append(dma("sp", xo_d[g0:g0 + 128, :], xt, rd=[b_tr[s]]).idx)
                yb = MIXTAB[:, 0:1024]
                act(yb, xt, AF.Square, [b_tr[s]], [b_acc[0], b_acc[1], b_sm], accum_out=small[:, 0:1])
                ts("dve", small[:, 1:2], small[:, 0:1], 1.0 / D, EPS, ALU.mult, ALU.add, [b_sm], [b_sm])
                rsq(small[:, 1:2], [b_sm], [b_sm])
                act(yb, xt, AF.Identity, [b_tr[s], b_sm], [b_acc[0], b_acc[1]], scale=small[:, 1:2])
                tt("pool", yb, yb, fnwbc[:], ALU.mult, [b_acc[0], b_acc[1], b_misc], [b_acc[0], b_acc[1]])
                final_waits.append(dma("sp", yo_d[g0:g0 + 128, :], yb, rd=[b_acc[0], b_acc[1]]).idx)
            P.barrier()
        final_waits.append(dma("sp", ahalo_o, ahalo[:], rd=[b_ah]).idx)
        P.emit(es, final_waits)
    return nc


_PROG = {}
_CST = {}


def _prog(L):
    if L not in _PROG:
        _PROG[L] = build_seg(L)
    return _PROG[L]


def _layer_maps(inp, l):
    f = lambda a: np.ascontiguousarray(a, dtype=np.float32)
    ab = np.asarray(inp["ada_b"][l]).reshape(6, 8, 128)
    sel = np.zeros((1, 4), np.float32)
    sel[0, 1:l + 1] = 1.0
    return dict(
        w_in=f(inp["w_in"][l]), w_out=f(inp["w_out"][l]), w_gate=f(inp["w_gate"][l]), w_val=f(inp["w_val"][l]),
        w_down=f(inp["w_down"][l]), ada_w=f(inp["ada_w"][l]), ada_b=f(np.asarray(inp["ada_b"][l])[None, :]),
        adabcol=f(ab[[0, 1, 3, 4, 2, 5]].transpose(2, 0, 1).reshape(128, 48)),
        n1col=f(np.asarray(inp["norm1_w"][l]).reshape(8, 128).T), n2col=f(np.asarray(inp["norm2_w"][l]).reshape(8, 128).T),
        retnw=f(np.asarray(inp["ret_norm_w"][l])[None, :]), hgnw=f(np.asarray(inp["hgrn_norm_w"][l])[None, :]),
        fnw=f(np.asarray(inp["final_norm_w"])[None, :]), lbl=f(np.asarray(inp["hgrn_lb_logits"]).reshape(1, 2048)), sel=sel,
        convw=f(np.asarray(inp["conv_w"][l]).reshape(3, NFC, 128).transpose(2, 1, 0).reshape(128, NFC * 3)),
        convb=f(np.asarray(inp["conv_b"][l]).reshape(NFC, 128).T),
    )


def run_wavefront(inp, L, nseg):
    if "cst" not in _CST:
        _CST["cst"] = _consts()
        _CST["cos"], _CST["sin"] = _rope_tables()
    x = np.asarray(inp["x"], dtype=np.float32)
    c = np.asarray(inp["c"], dtype=np.float32)
    Bn = x.shape[0]
    nc = _prog(L)
    lm = [_layer_maps(inp, l) for l in range(DEPTH)]
    X = {}
    ST = {}
    z512 = np.zeros((128, 512), np.float32)
    zh = np.zeros((128, NFC * 2), np.float32)
    out = np.zeros((Bn, nseg * L, D), np.float32)
    for b in range(Bn):
        for j in range(nseg):
            X[(b, j, 0)] = np.ascontiguousarray(x[b, j * L:(j + 1) * L])
    import os
    for t in range(1 if os.environ.get('KONE') else nseg + DEPTH - 1):
        jobs = [(b, j, t - j) for b in range(Bn) for j in range(nseg) if 0 <= t - j < DEPTH]
        maps = []
        for (b, j, l) in jobs:
            m = dict(lm[l])
            m["x"] = X[(b, j, l)]
            m["ccol"] = np.ascontiguousarray(c[b].reshape(8, 128).T)
            st = ST.get((b, j - 1, l))
            m["sret"], m["shg"], m["ahalo"] = st if st is not None else (z512, z512, zh)
            m["cos"] = np.ascontiguousarray(_CST["cos"][j * L:(j + 1) * L])
            m["sin"] = np.ascontiguousarray(_CST["sin"][j * L:(j + 1) * L])
            m["cst"] = _CST["cst"]
            maps.append(m)
        res = run_bass_kernel_spmd(nc, maps, core_ids=list(range(len(maps))))
        for (b, j, l), r in zip(jobs, res.results):
            ST[(b, j, l)] = (r["sret_o"], r["shg_o"], r["ahalo_o"])
            if l + 1 < DEPTH:
                X[(b, j, l + 1)] = r["xo"]
            else:
                out[b, j * L:(j + 1) * L] = r["yo"]
            X.pop((b, j, l), None)
    return out


def kernel(**inputs):
    return run_wavefront(inputs, LSEG, NSEG)
```
